# Optimizing a Trainium2 kernel written in Bass

```python
import jax, jax.numpy as jnp
from jax import lax
import numpy as np

D_MODEL = 2048
BATCH = 8
SEQ = 4096
DEPTH = 1
DEC_BATCH = 8
DEC_SEQ = 16
PAST_LEN = 2048

CHUNK = 64
HEAD_DIM = 64
ATTN_WIDTH = D_MODEL // 2
CONV_CH = D_MODEL - ATTN_WIDTH
N_HEADS = ATTN_WIDTH // HEAD_DIM
N_KV_HEADS = N_HEADS // 4
GQA_GROUP = N_HEADS // N_KV_HEADS
ROT_DIM = HEAD_DIM // 4
ROPE_THETA = 500000.0
WINDOW = 128
WINDOW_CHUNKS = WINDOW // CHUNK
CONV_WIDTH = 31
N_EXPERTS = 32
TOP_K = 4
D_FF = D_MODEL
SWIGLU_LIMIT = 7.0
SWIGLU_ALPHA = 1.702
MOE_BLOCK = 256
EPS = 1e-5
NEG_INF = -1e30
Q_COLS = N_HEADS * HEAD_DIM
KV_COLS = N_KV_HEADS * HEAD_DIM
IN_COLS = Q_COLS + 2 * KV_COLS + 2 * CONV_CH

kernel_name = 'hybrid_swa_sink_conformer_moe_stream_step'


def _rmsnorm(x, g):
    xf = x.astype(jnp.float32)
    y = xf * lax.rsqrt(jnp.mean(xf * xf, axis=-1, keepdims=True) + EPS)
    return (y * g.astype(jnp.float32)).astype(x.dtype)


def _layernorm(x, g, b):
    xf = x.astype(jnp.float32)
    mu = jnp.mean(xf, axis=-1, keepdims=True)
    var = jnp.mean(jnp.square(xf - mu), axis=-1, keepdims=True)
    y = (xf - mu) * lax.rsqrt(var + EPS)
    return (y * g.astype(jnp.float32) + b.astype(jnp.float32)).astype(x.dtype)


def _rotary(x, pos):
    half = ROT_DIM // 2
    inv_freq = ROPE_THETA ** (-jnp.arange(0, ROT_DIM, 2, dtype=jnp.float32) / ROT_DIM)
    ang = pos.astype(jnp.float32)[:, None] * inv_freq[None, :]
    cos = jnp.cos(ang)[None, :, None, :]
    sin = jnp.sin(ang)[None, :, None, :]
    xr = x[..., :ROT_DIM].astype(jnp.float32)
    x1, x2 = xr[..., :half], xr[..., half:]
    rot = jnp.concatenate([x1 * cos - x2 * sin, x2 * cos + x1 * sin], axis=-1)
    return jnp.concatenate([rot.astype(x.dtype), x[..., ROT_DIM:]], axis=-1)


def _modulation(c, w_ada, b_ada):
    m = jax.nn.silu(c) @ w_ada + b_ada
    return m.reshape(c.shape[0], 6, 1, D_MODEL)


def _mixer_inputs(x, shift, scale, g_mix, w_in, pos):
    b, s, _ = x.shape
    h = _rmsnorm(x, g_mix) * (1 + scale) + shift
    z = h @ w_in
    q = z[..., :Q_COLS].reshape(b, s, N_HEADS, HEAD_DIM)
    k = z[..., Q_COLS:Q_COLS + KV_COLS].reshape(b, s, N_KV_HEADS, HEAD_DIM)
    v = z[..., Q_COLS + KV_COLS:Q_COLS + 2 * KV_COLS].reshape(b, s, N_KV_HEADS, HEAD_DIM)
    o = Q_COLS + 2 * KV_COLS
    u = z[..., o:o + CONV_CH] * jax.nn.sigmoid(z[..., o + CONV_CH:])
    return _rotary(q, pos), _rotary(k, pos), v, u


def _sink_softmax(sc, sink):
    sk = jnp.broadcast_to(sink.astype(jnp.float32).reshape(N_KV_HEADS, GQA_GROUP, 1, 1), sc.shape[:-1] + (1,))
    p = jax.nn.softmax(jnp.concatenate([sc, sk], axis=-1), axis=-1)
    return p[..., :-1]


def _band_attention(q, k, v, sink):
    b, s = q.shape[0], q.shape[1]
    n_c = s // CHUNK
    qb = q.reshape(b, n_c, CHUNK, N_KV_HEADS, GQA_GROUP, HEAD_DIM)
    pad = WINDOW_CHUNKS * CHUNK

    def band(t):
        tp = jnp.pad(t, ((0, 0), (pad, 0), (0, 0), (0, 0))).reshape(b, n_c + WINDOW_CHUNKS, CHUNK, N_KV_HEADS, HEAD_DIM)
        return jnp.concatenate([tp[:, j:j + n_c] for j in range(WINDOW_CHUNKS + 1)], axis=2)

    kb, vb = band(k), band(v)
    key_chunk = jnp.arange(n_c)[:, None] + jnp.arange(WINDOW_CHUNKS + 1)[None, :] - WINDOW_CHUNKS
    valid = jnp.repeat(key_chunk >= 0, CHUNK, axis=1)
    sc = jnp.einsum('bnqkgd,bnskd->bnkgqs', qb, kb, preferred_element_type=jnp.float32) * (HEAD_DIM ** -0.5)
    sc = jnp.where(valid[None, :, None, None, None, :], sc, NEG_INF)
    p = _sink_softmax(sc, sink).astype(v.dtype)
    o = jnp.einsum('bnkgqs,bnskd->bnqkgd', p, vb)
    return o.reshape(b, s, Q_COLS)


def _cached_attention(q, k, v, cache_k, cache_v, sink):
    b, s = q.shape[0], q.shape[1]
    qg = q.reshape(b, s, N_KV_HEADS, GQA_GROUP, HEAD_DIM)
    k_all = jnp.concatenate([cache_k, k], axis=1)
    v_all = jnp.concatenate([cache_v, v], axis=1)
    sc = jnp.einsum('bqkgd,bskd->bkgqs', qg, k_all, preferred_element_type=jnp.float32) * (HEAD_DIM ** -0.5)
    p = _sink_softmax(sc, sink).astype(v.dtype)
    o = jnp.einsum('bkgqs,bskd->bqkgd', p, v_all)
    return o.reshape(b, s, Q_COLS)


def _conv_tail(u_hist, conv_w, conv_b, ln_g, ln_b):
    y = lax.conv_general_dilated(u_hist, conv_w, window_strides=(1,), padding='VALID',
                                 dimension_numbers=('NWC', 'WIO', 'NWC'),
                                 feature_group_count=CONV_CH) + conv_b
    return jax.nn.silu(_layernorm(y, ln_g, ln_b))


def _moe(h, w_router, b_router, w_gu, b_gu, w_down, b_down):
    b, s, d = h.shape
    t = b * s
    xf = h.reshape(t, d)
    logits = (xf @ w_router + b_router).astype(jnp.float32)
    top_v, top_i = lax.top_k(logits, TOP_K)
    gates = jax.nn.softmax(top_v, axis=-1).astype(h.dtype)
    tk = t * TOP_K
    blk = max(8, min(MOE_BLOCK, tk // N_EXPERTS))
    n_blk = -(-tk // blk) + N_EXPERTS
    flat_e = top_i.reshape(tk)
    flat_tok = jnp.arange(tk, dtype=jnp.int32) // TOP_K
    flat_g = gates.reshape(tk)
    order = jnp.argsort(flat_e)
    se = flat_e[order]
    counts = jnp.bincount(flat_e, length=N_EXPERTS)
    starts = jnp.cumsum(counts) - counts
    padded = (counts + blk - 1) // blk * blk
    pends = jnp.cumsum(padded)
    pstarts = pends - padded
    dest = pstarts[se] + jnp.arange(tk, dtype=jnp.int32) - starts[se]
    rows = n_blk * blk
    row_tok = jnp.full((rows,), t, dtype=jnp.int32).at[dest].set(flat_tok[order])
    row_gate = jnp.zeros((rows,), h.dtype).at[dest].set(flat_g[order])
    block_e = jnp.minimum(jnp.searchsorted(pends, jnp.arange(n_blk, dtype=jnp.int32) * blk, side='right'), N_EXPERTS - 1)
    xpad = jnp.concatenate([xf, jnp.zeros((1, d), xf.dtype)], axis=0)
    xin = xpad[row_tok].reshape(n_blk, blk, d)

    def expert(args):
        xb, e = args
        gu = xb @ w_gu[e] + b_gu[e]
        g, u = gu[:, :D_FF], gu[:, D_FF:]
        g = jnp.minimum(g, SWIGLU_LIMIT)
        u = jnp.clip(u, -SWIGLU_LIMIT, SWIGLU_LIMIT)
        a = g * jax.nn.sigmoid(SWIGLU_ALPHA * g) * (u + 1)
        return a @ w_down[e] + b_down[e]

    out = lax.map(expert, (xin, block_e)).reshape(rows, d)
    y = jnp.zeros((t + 1, d), h.dtype).at[row_tok].add(out * row_gate[:, None])
    return y[:t].reshape(b, s, d)


def _finish(x, attn, conv, gate1, shift2, scale2, gate2, w_out, g_ffn,
            w_router, b_router, w_gu, b_gu, w_down, b_down):
    x = x + gate1 * (jnp.concatenate([attn, conv], axis=-1) @ w_out)
    h = _rmsnorm(x, g_ffn) * (1 + scale2) + shift2
    return x + gate2 * _moe(h, w_router, b_router, w_gu, b_gu, w_down, b_down)


def setup_inputs(seed: int = 0) -> dict:
    key = jax.random.key(seed)
    ks = jax.random.split(key, 32)
    f32 = jnp.float32

    def nrm(k, shape, std):
        return jax.random.normal(k, shape, f32) * std

    L = DEPTH
    D = D_MODEL
    win = min(WINDOW, PAST_LEN)
    return {
        'x_prompt': nrm(ks[0], (BATCH, SEQ, D), 1.0),
        'x_sample': nrm(ks[1], (DEC_BATCH, DEC_SEQ, D), 1.0),
        'c_prompt': nrm(ks[2], (BATCH, D), 1.0),
        'c_sample': nrm(ks[3], (DEC_BATCH, D), 1.0),
        'cache_k': nrm(ks[4], (L, DEC_BATCH, win, N_KV_HEADS, HEAD_DIM), 1.0),
        'cache_v': nrm(ks[5], (L, DEC_BATCH, win, N_KV_HEADS, HEAD_DIM), 1.0),
        'state_conv': nrm(ks[6], (L, DEC_BATCH, CONV_WIDTH - 1, CONV_CH), 0.5),
        'w_ada': nrm(ks[7], (L, D, 6 * D), 0.5 * D ** -0.5),
        'b_ada': nrm(ks[8], (L, 6 * D), 0.02),
        'g_mix': 1.0 + nrm(ks[9], (L, D), 0.02),
        'w_in': nrm(ks[10], (L, D, IN_COLS), D ** -0.5),
        'attn_sink': nrm(ks[11], (L, N_HEADS), 0.5),
        'conv_w': nrm(ks[12], (L, CONV_WIDTH, 1, CONV_CH), CONV_WIDTH ** -0.5),
        'conv_b': nrm(ks[13], (L, CONV_CH), 0.02),
        'conv_ln_g': 1.0 + nrm(ks[14], (L, CONV_CH), 0.02),
        'conv_ln_b': nrm(ks[15], (L, CONV_CH), 0.02),
        'w_out': nrm(ks[16], (L, ATTN_WIDTH + CONV_CH, D), (ATTN_WIDTH + CONV_CH) ** -0.5),
        'g_ffn': 1.0 + nrm(ks[17], (L, D), 0.02),
        'w_router': nrm(ks[18], (L, D, N_EXPERTS), D ** -0.5),
        'b_router': nrm(ks[19], (L, N_EXPERTS), 0.01),
        'w_gu': nrm(ks[20], (L, N_EXPERTS, D, 2 * D_FF), D ** -0.5),
        'b_gu': nrm(ks[21], (L, N_EXPERTS, 2 * D_FF), 0.02),
        'w_down': nrm(ks[22], (L, N_EXPERTS, D_FF, D), D_FF ** -0.5),
        'b_down': nrm(ks[23], (L, N_EXPERTS, D), 0.02),
        'g_final': 1.0 + nrm(ks[24], (D,), 0.02),
    }


def reference(x_prompt, x_sample, c_prompt, c_sample, cache_k, cache_v, state_conv,
              w_ada, b_ada, g_mix, w_in, attn_sink, conv_w, conv_b, conv_ln_g, conv_ln_b,
              w_out, g_ffn, w_router, b_router, w_gu, b_gu, w_down, b_down, g_final):
    hp, hs = x_prompt, x_sample
    pos_p = jnp.arange(x_prompt.shape[1], dtype=jnp.int32)
    pos_s = PAST_LEN + jnp.arange(x_sample.shape[1], dtype=jnp.int32)
    new_kp, new_vp, new_cp = [], [], []
    new_ks, new_vs, new_cs = [], [], []
    for l in range(DEPTH):
        conv_p = (conv_w[l], conv_b[l], conv_ln_g[l], conv_ln_b[l])
        moe_p = (w_router[l], b_router[l], w_gu[l], b_gu[l], w_down[l], b_down[l])
        m = _modulation(c_prompt, w_ada[l], b_ada[l])
        q, k, v, u = _mixer_inputs(hp, m[:, 0], m[:, 1], g_mix[l], w_in[l], pos_p)
        att = _band_attention(q, k, v, attn_sink[l])
        cv = _conv_tail(jnp.pad(u, ((0, 0), (CONV_WIDTH - 1, 0), (0, 0))), *conv_p)
        hp = _finish(hp, att, cv, m[:, 2], m[:, 3], m[:, 4], m[:, 5], w_out[l], g_ffn[l], *moe_p)
        keep = min(WINDOW, k.shape[1])
        new_kp.append(k[:, -keep:])
        new_vp.append(v[:, -keep:])
        new_cp.append(u[:, -(CONV_WIDTH - 1):])
        m = _modulation(c_sample, w_ada[l], b_ada[l])
        q, k, v, u = _mixer_inputs(hs, m[:, 0], m[:, 1], g_mix[l], w_in[l], pos_s)
        att = _cached_attention(q, k, v, cache_k[l], cache_v[l], attn_sink[l])
        u_hist = jnp.concatenate([state_conv[l], u], axis=1)
        cv = _conv_tail(u_hist, *conv_p)
        hs = _finish(hs, att, cv, m[:, 2], m[:, 3], m[:, 4], m[:, 5], w_out[l], g_ffn[l], *moe_p)
        new_ks.append(k)
        new_vs.append(v)
        new_cs.append(u_hist[:, -(CONV_WIDTH - 1):])
    y_prompt = _rmsnorm(hp, g_final)
    y_sample = _rmsnorm(hs, g_final)
    return (y_prompt, y_sample, jnp.stack(new_kp), jnp.stack(new_vp), jnp.stack(new_cp),
            jnp.stack(new_ks), jnp.stack(new_vs), jnp.stack(new_cs))
```

```python
import numpy as np
from contextlib import ExitStack
import ml_dtypes
import concourse.bass as bass
import concourse.mybir as mybir
from concourse.bass_utils import run_bass_kernel_spmd

F32 = mybir.dt.float32; BF16 = mybir.dt.bfloat16; I32 = mybir.dt.int32; U32 = mybir.dt.uint32
AF = mybir.ActivationFunctionType; ALU = mybir.AluOpType; AX = mybir.AxisListType

D = 2048; DC = 16; SEQ = 4096; NTP = 32; NS = 16; NTILE = 33
NE = 32; TOPK = 4; DFF = 2048; EPS = 1e-5
QC = 1024; KVC = 256; CCH = 1024; INC = 3584
CAP = 1536
NEGM = -1.0e30


class Res:
    __slots__ = ("name", "w", "r")
    def __init__(self, name):
        self.name = name; self.w = None; self.r = []


class Prog:
    ENG = ("pe", "act", "dve", "pool", "sp")
    def __init__(self, nc, stack):
        self.nc = nc
        self.e = {"pe": nc.tensor, "act": nc.scalar, "dve": nc.vector, "pool": nc.gpsimd, "sp": nc.sync}
        self.sem = {k: stack.enter_context(nc.semaphore("prog_" + k)) for k in ("pe", "act", "dve", "pool")}
        self.cnt = {k: 0 for k in self.sem}
        self.seen = {k: {} for k in self.ENG}
        self.dsem = {}
        self.dcur = {}
        self.stack = stack
        self.dma_tokens = []
        self.ev = {k: [] for k in self.ENG}

    def check(self):
        val = {}; pc = {k: 0 for k in self.ENG}
        progress = True
        while progress:
            progress = False
            for k in self.ENG:
                while pc[k] < len(self.ev[k]):
                    kind, name, v = self.ev[k][pc[k]]
                    if kind == "w":
                        if val.get(name, 0) < v: break
                    else:
                        val[name] = val.get(name, 0) + v
                    pc[k] += 1; progress = True
        stuck = {k: (pc[k], len(self.ev[k]), self.ev[k][pc[k]], val.get(self.ev[k][pc[k]][1], 0)) for k in self.ENG if pc[k] < len(self.ev[k])}
        return stuck

    def _wait(self, eng, tok):
        sem, val = tok
        if sem.name in self.dcur:
            val = max(val, self.dcur[sem.name][1])
        if self.seen[eng].get(sem.name, 0) >= val:
            return
        self.seen[eng][sem.name] = val
        self.ev[eng].append(("w", sem.name, val))
        self.e[eng].wait_ge(sem, val)

    def deps(self, eng, reads, writes, skip_self=False):
        own = self.sem.get(eng)
        for r in reads:
            if r.w is not None and not (skip_self and r.w[0] is own):
                self._wait(eng, r.w)
        for w in writes:
            if w.w is not None and not (skip_self and w.w[0] is own):
                self._wait(eng, w.w)
            for t in w.r:
                if not (skip_self and t[0] is own):
                    self._wait(eng, t)

    def mark(self, tok, reads, writes):
        for r in reads:
            r.r = [t for t in r.r if t[0] is not tok[0]] + [tok]
        for w in writes:
            w.w = tok; w.r = []

    def op(self, eng, fn, reads=(), writes=(), inc=True):
        self.deps(eng, reads, writes, skip_self=(eng == "pe"))
        ins = fn()
        if inc:
            self.cnt[eng] += 1
            ins.then_inc(self.sem[eng], 1)
            self.ev[eng].append(("i", self.sem[eng].name, 1))
            tok = (self.sem[eng], self.cnt[eng])
        else:
            tok = (self.sem[eng], self.cnt[eng] + 1)
        self.mark(tok, reads, writes)
        return tok

    def _slot(self, slot):
        if slot not in self.dsem:
            self.dsem[slot] = [self.stack.enter_context(self.nc.semaphore("dma_" + slot)), 0]
            self.dcur[self.dsem[slot][0].name] = self.dsem[slot]
        return self.dsem[slot]

    def dma(self, q, slot, out, in_, reads=(), writes=(), **kw):
        self.deps(q, reads, writes)
        d = self._slot(slot)
        d[1] += 16
        self.e[q].dma_start(out=out, in_=in_, **kw).then_inc(d[0], 16)
        self.ev[q].append(("i", d[0].name, 16))
        tok = (d[0], d[1])
        self.mark(tok, reads, writes)
        self.dma_tokens.append(tok)
        return tok

    def idma(self, slot, out, out_off, in_, in_off, reads=(), writes=(), **kw):
        self.deps("pool", reads, writes)
        d = self._slot(slot)
        d[1] += 16
        self.nc.gpsimd.indirect_dma_start(out=out, out_offset=out_off, in_=in_, in_offset=in_off, **kw).then_inc(d[0], 16)
        self.ev["pool"].append(("i", d[0].name, 16))
        tok = (d[0], d[1])
        self.mark(tok, reads, writes)
        self.dma_tokens.append(tok)
        return tok

    def barrier(self):
        toks = [(self.sem[k], self.cnt[k]) for k in self.sem if self.cnt[k] > 0]
        latest = {}
        for s, v in self.dma_tokens:
            if s.name not in latest or latest[s.name][1] < v:
                latest[s.name] = (s, v)
        toks += list(latest.values())
        for eng in self.ENG:
            for t in toks:
                self._wait(eng, t)
        self.dma_tokens = list(latest.values())


def build(cap=CAP, nphase=4, dbg=False, tlim=NTILE, slim=99, small_w=False):
    nc = bass.Bass("TRN2", target_bir_lowering=False)
    def din(name, shape, dt=F32):
        return nc.dram_tensor(name, list(shape), dt, kind="ExternalInput").ap()
    def dout(name, shape, dt=F32):
        return nc.dram_tensor(name, list(shape), dt, kind="ExternalOutput").ap()
    def dint(name, shape, dt=F32):
        return nc.dram_tensor(name, list(shape), dt, kind="Internal").ap()

    xp = din("xp", [SEQ, D]); xsm = din("xsm", [NS, D]); c2T = din("c2T", [128, DC, 2])
    ck = din("ck", [128, 256]); cvv = din("cvv", [128, 256]); sconvT = din("sconvT", [128, 8, 30])
    w_ada = din("w_ada", [D, 6 * D]); bada2 = din("bada2", [2, 6 * D])
    gmix2 = din("gmix2", [2, D]); gffn2 = din("gffn2", [2, D]); gfin = din("gfin", [1, D])
    w_in = din("w_in", [D, INC]); w_out = din("w_out", [D, D])
    if small_w:
        w_gu = din("w_gu", [1, 8, 8]); w_down = din("w_down", [1, 8, 8])
    else:
        w_gu = din("w_gu", [NE, D, 2 * DFF]); w_down = din("w_down", [NE, DFF, D])
    bguT = din("bguT", [128, NE, 32]); b_down = din("b_down", [NE, D])
    wrT = din("wrT", [128, DC, NE]); brow = din("brow", [1, NE])
    sinkbc = din("sinkbc", [128, 16]); cwT = din("cwT", [128, 8, 31]); convp = din("convp", [128, 3, 8])
    identF_d = din("identF", [128, 128]); identB_d = din("identB", [128, 128], BF16)
    onesF_d = din("onesF", [128, 128]); triF_d = din("triF", [128, 128])
    iotaE_d = din("iotaE", [128, NE]); iotaC_d = din("iotaC", [128, NE])
    masks_d = din("masks", [128, 4, 256], BF16); ropeT_d = din("ropeT", [128, NTILE, 16])

    y_p = dout("y_p", [SEQ, D]); y_s = dout("y_s", [NS, D])
    nkp = dout("nkp", [128, 256]); nvp = dout("nvp", [128, 256]); ncp = dout("ncp", [30, CCH])
    nks = dout("nks", [NS, 256]); nvs = dout("nvs", [NS, 256]); ncs = dout("ncs", [30, CCH])

    dscr = dout if dbg else dint
    MODS = dscr("MODS", [2, 6 * D])
    ACs = dscr("ACs", [NTILE, 128, 16, 128], BF16)
    X1 = dscr("X1", [SEQ + 128, D])
    XS = dint("XS", [NE * cap, D], BF16)
    YSh = [dint("YS%d" % i, [NE * cap, D // 2]) for i in range(2)]
    dbgo = {}
    if dbg:
        dbg_dest = dout('dbg_dest', [128, NTILE * 4], I32); dbg_gate = dout('dbg_gate', [128, NTILE * 4])
    _PG = []

    def xrows(t):
        return (xp[t * 128:(t + 1) * 128, :], 128, 0) if t < NTP else (xsm[:, :], NS, 1)

    with ExitStack() as top:
        pg = Prog(nc, top); _PG.append(pg); dbgo['pg'] = pg
        def SB(st, n, s, d=F32): return st.enter_context(nc.sbuf_tensor("s_" + n, list(s), d))
        def PS(st, n, s, d=F32): return st.enter_context(nc.psum_tensor("p_" + n, list(s), d))
        V = nc.vector; A = nc.scalar; T = nc.tensor; G = nc.gpsimd
        bcreg = G.alloc_register('bcreg'); G.reg_mov(bcreg, NE * cap - 1); BCV = G.snap(bcreg)

        DEST = SB(top, "DEST", [128, NTILE, 4], I32); rDEST = Res("DEST")
        GATES = SB(top, "GATES", [128, NTILE, 4]); rGATES = Res("GATES")
        identF = SB(top, "identF_s", [128, 128]); identB = SB(top, "identB_s", [128, 128], BF16)
        onesF = SB(top, "onesF_s", [128, 128]); rC = Res("consts")
        pg.dma("sp", "c0", identF[:], identF_d, writes=[rC])
        pg.dma("sp", "c0", identB[:], identB_d, writes=[rC])
        pg.dma("sp", "c0", onesF[:], onesF_d, writes=[rC])

        pD = [PS(top, "pD%d" % i, [128, 1024]) for i in range(3)]; rpD = [Res("pD%d" % i) for i in range(3)]
        pB = [PS(top, "pB%d" % i, [128, 1024], BF16) for i in range(2)]; rpB = [Res("pB%d" % i) for i in range(2)]
        ring = {"d": 0, "b": 0}
        def nextD():
            i = ring["d"] % 3; ring["d"] += 1; return pD[i], rpD[i]
        def nextB():
            i = ring["b"] % 2; ring["b"] += 1; return pB[i], rpB[i]

        with ExitStack() as s0:
            c2f = SB(s0, "c2f", [128, DC, 2]); c2s = SB(s0, "c2s", [128, DC, 2], BF16)
            mods = SB(s0, "mods", [2, 6 * D]); rmods = Res("mods")
            gm2 = SB(s0, "gm2", [2, D]); gf2 = SB(s0, "gf2", [2, D]); rg = Res("g2")
            wsl = [SB(s0, "wada%d" % i, [128, DC, 512], BF16) for i in range(2)]; rw = [Res("wada%d" % i) for i in range(2)]
            bsl = [SB(s0, "bada%d" % i, [2, 512]) for i in range(2)]; rb = [Res("bada%d" % i) for i in range(2)]
            rc2 = Res("c2")
            pg.dma("sp", "c2", c2f[:], c2T, writes=[rc2])
            pg.dma("sp", "c2g", gm2[:], gmix2, writes=[rg])
            pg.dma("sp", "c2g", gf2[:], gffn2, writes=[rg])
            pg.op("act", lambda: A.activation(c2s[:], c2f[:], AF.Silu), reads=[rc2], writes=[rc2])
            wv = w_ada.rearrange("(kc p) n -> p kc n", p=128)
            for nb in range(24):
                sl = nb % 2
                pg.dma("pool", "wada%d" % sl, wsl[sl][:], wv[:, :, nb * 512:(nb + 1) * 512], writes=[rw[sl]])
                pg.dma("sp", "bada%d" % sl, bsl[sl][:], bada2[:, nb * 512:(nb + 1) * 512], writes=[rb[sl]])
                pm, rpm = nextD()
                for kc in range(DC):
                    pg.op("pe", lambda: T.matmul(pm[0:2, 0:512], c2s[:, kc, :], wsl[sl][:, kc, :], start=(kc == 0), stop=(kc == DC - 1)),
                          reads=[rc2, rw[sl]], writes=[rpm], inc=(kc == DC - 1))
                pg.op("dve", lambda: V.tensor_tensor(mods[:, nb * 512:(nb + 1) * 512], pm[0:2, 0:512], bsl[sl][:], op=ALU.add),
                      reads=[rpm, rb[sl]], writes=[rmods])
            pg.op("dve", lambda: V.scalar_tensor_tensor(mods[:, D:2 * D], mods[:, D:2 * D], 1.0, gm2[:], op0=ALU.add, op1=ALU.mult),
                  reads=[rmods, rg], writes=[rmods])
            pg.op("dve", lambda: V.scalar_tensor_tensor(mods[:, 4 * D:5 * D], mods[:, 4 * D:5 * D], 1.0, gf2[:], op0=ALU.add, op1=ALU.mult),
                  reads=[rmods, rg], writes=[rmods])
            rMODS = Res("MODS")
            pg.dma("sp", "mods", MODS, mods[:], reads=[rmods], writes=[rMODS])
        pg.barrier()
        if nphase < 1:
            return nc, dbgo

        def mrow(s, i):
            return MODS[s:s + 1, i * D:(i + 1) * D]

        with ExitStack() as s1:
            Win = SB(s1, "Win", [128, DC, INC], BF16); rWin = Res("Win")
            wiv = w_in.rearrange("(kc p) n -> p kc n", p=128)
            for c in range(7):
                pg.dma("pool", "win", Win[:, :, c * 512:(c + 1) * 512], wiv[:, :, c * 512:(c + 1) * 512], writes=[rWin])
            G1T = SB(s1, "G1T", [128, 2, DC]); S1T = SB(s1, "S1T", [128, 2, DC]); rGS = Res("GS")
            for s in range(2):
                pg.dma("sp", "gs", G1T[:, s, :], mrow(s, 1).rearrange("o (dc p) -> p (o dc)", p=128), reads=[rMODS], writes=[rGS], allow_slow_non_contiguous=True)
                pg.dma("sp", "gs", S1T[:, s, :], mrow(s, 0).rearrange("o (dc p) -> p (o dc)", p=128), reads=[rMODS], writes=[rGS], allow_slow_non_contiguous=True)
            masks = SB(s1, "masks", [128, 4, 256], BF16); ropeT = SB(s1, "ropeT", [128, NTILE, 16])
            sink = SB(s1, "sink", [128, 16]); cw = SB(s1, "cw", [128, 8, 31]); cpar = SB(s1, "cpar", [128, 3, 8])
            for dst, src in ((masks, masks_d), (ropeT, ropeT_d), (sink, sinkbc), (cw, cwT), (cpar, convp)):
                pg.dma("sp", "c1", dst[:], src, writes=[rC])
            xb = [SB(s1, "xb%d" % i, [128, D]) for i in range(2)]; rxb = [Res("xb%d" % i) for i in range(2)]
            xn = SB(s1, "xn", [128, D]); rxn = Res("xn")
            st1 = SB(s1, "st1", [128, 8]); rst1 = Res("st1")
            hT = SB(s1, "hT", [128, DC, 128], BF16); rhT = Res("hT")
            qsb = SB(s1, "qsb", [128, QC], BF16); rqsb = Res("qsb")
            kvf = SB(s1, "kvf", [128, 512]); rkvf = Res("kvf")
            kdup = SB(s1, "kdup", [128, 4, 2, 64], BF16); rkdup = Res("kdup")
            rt4 = [SB(s1, "rt%d" % i, [128, 16, 8]) for i in range(4)]; rrt = Res("rt")
            QT = SB(s1, "QT", [128, 8, 128], BF16); rQT = Res("QT")
            KT2 = SB(s1, "KT2", [128, 4, 2, 128], BF16); rKT = [Res("KT0"), Res("KT1")]
            Vlo = SB(s1, "Vlo", [128, 2, 4, 128], BF16); Vhi = SB(s1, "Vhi", [128, 2, 4, 128], BF16); rVV = [Res("V0"), Res("V1")]
            Pf0 = SB(s1, "Pf0", [128, 4, 256]); Pf = [Pf0, Pf0]; rPf0 = Res("Pf0"); rPf = [rPf0, rPf0]
            Pn = [SB(s1, "Pn%d" % i, [128, 4, 256], BF16) for i in range(2)]; rPn = [Res("Pn%d" % i) for i in range(2)]
            PT = [SB(s1, "PT%d" % i, [128, 8, 128], BF16) for i in range(2)]; rPT = [Res("PT%d" % i) for i in range(2)]
            sm = [SB(s1, "sm%d" % i, [128, 24]) for i in range(2)]; rsm = [Res("sm%d" % i) for i in range(2)]
            acT = [SB(s1, "acT%d" % i, [128, 16, 128], BF16) for i in range(2)]; racT = [Res("acT%d" % i) for i in range(2)]
            sg = [SB(s1, "sg%d" % i, [128, 4, 128]) for i in range(2)]; rsg = [Res("sg%d" % i) for i in range(2)]
            uh = SB(s1, "uh", [128, 8, 30 + 128]); ruh = Res("uh")
            yT = SB(s1, "yT", [128, 8, 128]); ryT = Res("yT")
            ysq = SB(s1, "ysq", [128, 8, 128]); rysq = Res("ysq")
            lnst = SB(s1, "lnst", [128, 4, 128]); rln = Res("lnst")
            ncv = xn[0:30, 0:CCH]; rncv = rxn
            ckf = ysq[:].rearrange("p a b -> p (a b)")[:, 0:512]; rckf = rysq

            pg.op("dve", lambda: V.memset(uh[:], 0.0), writes=[ruh])
            pg.op("dve", lambda: V.memset(KT2[:], 0.0), writes=rKT)
            pg.op("dve", lambda: V.memset(Vlo[:], 0.0), writes=rVV)
            pg.op("dve", lambda: V.memset(Vhi[:], 0.0), writes=rVV)
            xtok = {}
            def load_x(t):
                src, nt, s = xrows(t)
                xtok[t] = pg.dma("sp", "xb%d" % (t % 2), xb[t % 2][:nt, :], src, writes=[rxb[t % 2]])
            load_x(0)
            for t in range(min(NTILE, tlim)):
                src, nt, s = xrows(t)
                hb = t % 2; ob = 1 - hb
                mvar = 2 if t == 0 else (3 if t == NTP else (0 if hb == 0 else 1))
                if t + 1 < NTILE:
                    load_x(t + 1)
                x = xb[t % 2]; rx = rxb[t % 2]
                pg.op("dve", lambda: V.memset(st1[:nt, 0:1], 0.0), writes=[rst1])
                pg.op("act", lambda: A.activation(xn[:nt, :], x[:nt, :], AF.Square, accum_out=st1[:nt, 0:1]), reads=[rx], writes=[rxn, rst1])
                pg.op("act", lambda: A.activation(st1[:nt, 1:2], st1[:nt, 0:1], AF.Sqrt, bias=EPS, scale=1.0 / D), reads=[rst1], writes=[rst1])
                pg.op("dve", lambda: V.reciprocal(st1[:nt, 2:3], st1[:nt, 1:2]), reads=[rst1], writes=[rst1])
                pg.op("act", lambda: A.activation(xn[:nt, :], x[:nt, :], AF.Copy, scale=st1[:nt, 2:3]), reads=[rx, rst1], writes=[rxn])
                for half in range(2):
                    pt, rpt = nextD()
                    for b8 in range(8):
                        dc = half * 8 + b8
                        pg.op("pe", lambda: T.transpose(pt[:, b8 * 128:b8 * 128 + nt], xn[:nt, dc * 128:(dc + 1) * 128], identF[:nt, :nt]),
                              reads=[rxn, rC], writes=[rpt], inc=(b8 == 7))
                    for b8 in range(8):
                        dc = half * 8 + b8
                        pg.op("dve", lambda: V.tensor_scalar(hT[:, dc, :nt], pt[:, b8 * 128:b8 * 128 + nt], G1T[:, s, dc:dc + 1], S1T[:, s, dc:dc + 1], op0=ALU.mult, op1=ALU.add),
                              reads=[rpt, rGS], writes=[rhT])
                if slim < 1: continue
                pq, rpq = nextD()
                for g in range(2):
                    for dc in range(DC):
                        pg.op("pe", lambda: T.matmul(pq[:nt, g * 512:(g + 1) * 512], hT[:, dc, :nt], Win[:, dc, g * 512:(g + 1) * 512], start=(dc == 0), stop=(dc == DC - 1)),
                              reads=[rhT, rWin], writes=[rpq], inc=(dc == DC - 1))
                pk, rpk = nextD()
                for dc in range(DC):
                    pg.op("pe", lambda: T.matmul(pk[:nt, 0:512], hT[:, dc, :nt], Win[:, dc, 1024:1536], start=(dc == 0), stop=(dc == DC - 1)),
                          reads=[rhT, rWin], writes=[rpk], inc=(dc == DC - 1))
                for g in range(2):
                    pg.op("act", lambda: A.copy(qsb[:nt, g * 512:(g + 1) * 512], pq[:nt, g * 512:(g + 1) * 512]), reads=[rpq], writes=[rqsb])
                pg.op("act", lambda: A.copy(kvf[:nt, :], pk[:nt, 0:512]), reads=[rpk], writes=[rkvf])
                if slim < 2: continue
                cosq = ropeT[:nt, t:t + 1, 0:8].to_broadcast([nt, 16, 8]); sinq = ropeT[:nt, t:t + 1, 8:16].to_broadcast([nt, 16, 8])
                cosk = ropeT[:nt, t:t + 1, 0:8].to_broadcast([nt, 4, 8]); sink_ = ropeT[:nt, t:t + 1, 8:16].to_broadcast([nt, 4, 8])
                for g in range(2):
                    pg.op("act", lambda: A.copy(xn[:nt, g * 512:(g + 1) * 512], pq[:nt, g * 512:(g + 1) * 512]), reads=[rpq], writes=[rxn])
                pq3 = xn[:nt, 0:QC].rearrange("p (h d) -> p h d", d=64); qs3 = qsb[:nt, :].rearrange("p (h d) -> p h d", d=64)
                kf3 = kvf[:nt, 0:256].rearrange("p (h d) -> p h d", d=64)
                for ri, (src3, dst3, nh, cs, sn, rsrc, rdst) in enumerate(((pq3, qs3, 16, cosq, sinq, rxn, rqsb), (kf3, kf3, 4, cosk, sink_, rkvf, rkvf))):
                    a_, b_, c_, d_ = [r[:nt, 0:nh, :] for r in rt4]
                    pg.op("dve", lambda: V.tensor_tensor(a_, src3[:, :, 0:8], cs, op=ALU.mult), reads=[rsrc, rC], writes=[rrt])
                    pg.op("dve", lambda: V.tensor_tensor(b_, src3[:, :, 8:16], sn, op=ALU.mult), reads=[rsrc, rC], writes=[rrt])
                    pg.op("dve", lambda: V.tensor_tensor(c_, src3[:, :, 8:16], cs, op=ALU.mult), reads=[rsrc, rC], writes=[rrt])
                    pg.op("dve", lambda: V.tensor_tensor(d_, src3[:, :, 0:8], sn, op=ALU.mult), reads=[rsrc, rC], writes=[rrt])
                    pg.op("dve", lambda: V.tensor_tensor(dst3[:, :, 0:8], a_, b_, op=ALU.subtract), reads=[rrt], writes=[rdst])
                    pg.op("dve", lambda: V.tensor_tensor(dst3[:, :, 8:16], c_, d_, op=ALU.add), reads=[rrt], writes=[rdst])
                if slim < 3: continue
                if t == NTP - 1:
                    pg.dma("sp", "okv", nkp, kvf[:, 0:256], reads=[rkvf])
                    pg.dma("sp", "okv", nvp, kvf[:, 256:512], reads=[rkvf])
                if t == NTP:
                    pg.dma("sp", "okv", nks, kvf[:NS, 0:256], reads=[rkvf])
                    pg.dma("sp", "okv", nvs, kvf[:NS, 256:512], reads=[rkvf])
                if t == NTP:
                    pg.dma("sp", "ckf", ckf[:, 0:256], ck, writes=[rckf])
                    pg.dma("sp", "ckf", ckf[:, 256:512], cvv, writes=[rckf])
                    ck3 = ckf[:, 0:256].rearrange("p (h d) -> p h d", d=64)
                    pg.op("pool", lambda: G.tensor_copy(kdup[:, :, 0, :], ck3), reads=[rckf], writes=[rkdup])
                    pg.op("pool", lambda: G.tensor_copy(kdup[:, :, 1, :], ck3), reads=[rckf], writes=[rkdup])
                    pb, rpb = nextB()
                    kd2 = kdup[:].rearrange("p g t d -> p (g t d)")
                    for g in range(4):
                        pg.op("pe", lambda: T.transpose(pb[:, g * 128:(g + 1) * 128], kd2[:, g * 128:(g + 1) * 128], identB[:, :]),
                              reads=[rkdup, rC], writes=[rpb], inc=(g == 3))
                    pg.op("act", lambda: A.copy(KT2[:, :, ob, :], pb[:, 0:512].rearrange("p (g k) -> p g k", k=128)), reads=[rpb], writes=[rKT[ob]])
                    cv3 = ckf[:, 256:512].rearrange("p (h d) -> p h d", d=64)
                    pg.op("pool", lambda: G.tensor_copy(Vlo[:, ob, :, 0:64], cv3), reads=[rckf], writes=[rVV[ob]])
                    pg.op("pool", lambda: G.tensor_copy(Vhi[:, ob, :, 64:128], cv3), reads=[rckf], writes=[rVV[ob]])
                    pg.op("dve", lambda: V.memset(KT2[:, :, hb, :], 0.0), writes=[rKT[hb]])
                    pg.op("dve", lambda: V.memset(Vlo[:, hb, :, 0:64], 0.0), writes=[rVV[hb]])
                    pg.op("dve", lambda: V.memset(Vhi[:, hb, :, 64:128], 0.0), writes=[rVV[hb]])
                pg.op("pool", lambda: G.tensor_copy(kdup[:nt, :, 0, :], kf3), reads=[rkvf], writes=[rkdup])
                pg.op("pool", lambda: G.tensor_copy(kdup[:nt, :, 1, :], kf3), reads=[rkvf], writes=[rkdup])
                vf3 = kvf[:nt, 256:512].rearrange("p (h d) -> p h d", d=64)
                pg.op("pool", lambda: G.tensor_copy(Vlo[:nt, hb, :, 0:64], vf3), reads=[rkvf], writes=[rVV[hb]])
                pg.op("pool", lambda: G.tensor_copy(Vhi[:nt, hb, :, 64:128], vf3), reads=[rkvf], writes=[rVV[hb]])
                pb, rpb = nextB()
                for b8 in range(8):
                    pg.op("pe", lambda: T.transpose(pb[:, b8 * 128:b8 * 128 + nt], qsb[:nt, b8 * 128:(b8 + 1) * 128], identB[:nt, :nt]),
                          reads=[rqsb, rC], writes=[rpb], inc=(b8 == 7))
                pg.op("act", lambda: A.copy(QT[:, :, :nt], pb[:, :].rearrange("p (b k) -> p b k", k=128)[:, :, :nt]), reads=[rpb], writes=[rQT])
                pb2, rpb2 = nextB()
                kd2 = kdup[:].rearrange("p g t d -> p (g t d)")
                for g in range(4):
                    pg.op("pe", lambda: T.transpose(pb2[:, g * 128:g * 128 + nt], kd2[:nt, g * 128:(g + 1) * 128], identB[:nt, :nt]),
                          reads=[rkdup, rC], writes=[rpb2], inc=(g == 3))
                pg.op("act", lambda: A.copy(KT2[:, :, hb, :nt], pb2[:, 0:512].rearrange("p (g k) -> p g k", k=128)[:, :, :nt]), reads=[rpb2], writes=[rKT[hb]])
                if slim < 4: continue
                for jj in range(2):
                    pv, rpv = nextD()
                    for jb in range(4):
                        j = jj * 4 + jb
                        for which, base in ((0, 1536), (1, 2560)):
                            for dc in range(DC):
                                pg.op("pe", lambda: T.matmul(pv[:, which * 512 + jb * 128: which * 512 + jb * 128 + nt], Win[:, dc, base + j * 128: base + (j + 1) * 128], hT[:, dc, :nt],
                                                             start=(dc == 0), stop=(dc == DC - 1)),
                                      reads=[rhT, rWin], writes=[rpv], inc=(dc == DC - 1))
                    sgt = sg[jj]; rsgt = rsg[jj]
                    pvv = pv[:, 0:512].rearrange("p (b k) -> p b k", k=128)[:, :, :nt]
                    pvg = pv[:, 512:1024].rearrange("p (b k) -> p b k", k=128)[:, :, :nt]
                    pg.op("act", lambda: A.activation(sgt[:, :, :nt], pvg, AF.Sigmoid), reads=[rpv], writes=[rsgt])
                    pg.op("dve", lambda: V.tensor_tensor(uh[:, jj * 4:(jj + 1) * 4, 30:30 + nt], pvv, sgt[:, :, :nt], op=ALU.mult), reads=[rpv, rsgt], writes=[ruh])
                if t == NTP:
                    pg.dma("sp", "sconv", uh[:, :, 0:30], sconvT, writes=[ruh])
                if slim < 5: continue
                ao = acT[t % 2]; rao = racT[t % 2]
                for g in range(4):
                    pi = g % 2
                    pS, rpS = nextD()
                    for j in range(4):
                        h = 4 * g + j; blk = h // 2; hf = h % 2
                        pg.op("pe", lambda: T.matmul(pS[:nt, j * 256:(j + 1) * 256], QT[hf * 64:(hf + 1) * 64, blk, :nt], KT2[hf * 64:(hf + 1) * 64, g, :, :].rearrange("p a k -> p (a k)"), start=True, stop=False),
                              reads=[rQT, rKT[0], rKT[1]], writes=[rpS], inc=False)
                        pg.op("pe", lambda: T.matmul(pS[:nt, j * 256:(j + 1) * 256], identB[:nt, :nt], masks[:nt, mvar, :], start=False, stop=True),
                              reads=[rC], writes=[rpS], inc=(j == 3))
                    sm_ = sm[pi]; rsm_ = rsm[pi]
                    pS3 = pS[:nt, :].rearrange("p (j k) -> p j k", k=256)
                    pg.op("dve", lambda: V.tensor_reduce(sm_[:nt, 0:4], pS3, axis=AX.X, op=ALU.max), reads=[rpS], writes=[rsm_])
                    pg.op("dve", lambda: V.scalar_tensor_tensor(sm_[:nt, 4:8], sm_[:nt, 0:4], 0.125, sink[:nt, 4 * g:4 * g + 4], op0=ALU.mult, op1=ALU.max), reads=[rsm_, rC], writes=[rsm_])
                    pg.op("dve", lambda: V.tensor_scalar(sm_[:nt, 4:8], sm_[:nt, 4:8], -1.0, None, op0=ALU.mult), reads=[rsm_], writes=[rsm_])
                    pg.op("dve", lambda: V.memset(sm_[:nt, 8:12], 0.0), writes=[rsm_])
                    for j in range(4):
                        pg.op("act", lambda: A.activation(Pf[pi][:nt, j, :], pS[:nt, j * 256:(j + 1) * 256], AF.Exp, bias=sm_[:nt, 4 + j:5 + j], scale=0.125, accum_out=sm_[:nt, 8 + j:9 + j]),
                              reads=[rpS, rsm_], writes=[rPf[pi], rsm_])
                    pg.op("dve", lambda: V.tensor_tensor(sm_[:nt, 12:16], sink[:nt, 4 * g:4 * g + 4], sm_[:nt, 4:8], op=ALU.add), reads=[rsm_, rC], writes=[rsm_])
                    pg.op("act", lambda: A.activation(sm_[:nt, 12:16], sm_[:nt, 12:16], AF.Exp), reads=[rsm_], writes=[rsm_])
                    pg.op("dve", lambda: V.tensor_tensor(sm_[:nt, 16:20], sm_[:nt, 8:12], sm_[:nt, 12:16], op=ALU.add), reads=[rsm_], writes=[rsm_])
                    pg.op("dve", lambda: V.reciprocal(sm_[:nt, 20:24], sm_[:nt, 16:20]), reads=[rsm_], writes=[rsm_])
                    for j in range(4):
                        pg.op("pool", lambda: G.tensor_scalar(Pn[pi][:nt, j, :], Pf[pi][:nt, j, :], sm_[:nt, 20 + j:21 + j], None, op0=ALU.mult), reads=[rPf[pi], rsm_], writes=[rPn[pi]])
                    pbt, rpbt = nextB()
                    for j in range(4):
                        for hf2 in range(2):
                            bi = j * 2 + hf2
                            pg.op("pe", lambda: T.transpose(pbt[:, bi * 128:bi * 128 + nt], Pn[pi][:nt, j, hf2 * 128:(hf2 + 1) * 128], identB[:nt, :nt]),
                                  reads=[rPn[pi], rC], writes=[rpbt], inc=(bi == 7))
                    pg.op("act", lambda: A.copy(PT[pi][:, :, :nt], pbt[:, :].rearrange("p (b k) -> p b k", k=128)[:, :, :nt]), reads=[rpbt], writes=[rPT[pi]])
                    po, rpo = nextD()
                    for pr in range(2):
                        oi = pr
                        seq = []
                        for hf2 in range(2):
                            seq.append((Vlo[:, hf2, g, :], PT[pi][:, (2 * pr) * 2 + hf2, :nt]))
                            seq.append((Vhi[:, hf2, g, :], PT[pi][:, (2 * pr + 1) * 2 + hf2, :nt]))
                        for k4, (lh, rh) in enumerate(seq):
                            pg.op("pe", lambda: T.matmul(po[:, oi * 128:oi * 128 + nt], lh, rh, start=(k4 == 0), stop=(k4 == 3)),
                                  reads=[rVV[0], rVV[1], rPT[pi]], writes=[rpo], inc=(k4 == 3))
                    pg.op("act", lambda: A.copy(ao[:, 2 * g:2 * g + 2, :nt], po[:, 0:256].rearrange("p (b k) -> p b k", k=128)[:, :, :nt]), reads=[rpo], writes=[rao])
                if slim < 6: continue
                for k in range(31):
                    for j in range(8):
                        eng, E = ("dve", V)
                        if k == 0:
                            pg.op(eng, lambda: E.tensor_scalar(yT[:, j, :nt], uh[:, j, 0:nt], cw[:, j, 0:1], cpar[:, 0, j:j + 1], op0=ALU.mult, op1=ALU.add),
                                  reads=[ruh, rC], writes=[ryT])
                        else:
                            pg.op(eng, lambda: E.scalar_tensor_tensor(yT[:, j, :nt], uh[:, j, k:k + nt], cw[:, j, k:k + 1], yT[:, j, :nt], op0=ALU.mult, op1=ALU.add),
                                  reads=[ruh, rC], writes=[ryT])
                if t == NTP - 1 or t == NTP:
                    off = 128 if t == NTP - 1 else NS
                    px, rpx = nextD()
                    for j in range(8):
                        pg.op("pe", lambda: T.transpose(px[0:30, j * 128:(j + 1) * 128], uh[:, j, off:off + 30], identF[:, :]), reads=[ruh, rC], writes=[rpx], inc=(j == 7))
                    pg.op("act", lambda: A.copy(ncv, px[0:30, :]), reads=[rpx], writes=[rncv])
                    pg.dma("sp", "oncv", ncp if t == NTP - 1 else ncs, ncv, reads=[rncv])
                if slim < 7: continue
                pg.op("act", lambda: A.activation(ysq[:, :, :nt], yT[:, :, :nt], AF.Square), reads=[ryT], writes=[rysq])
                pl, rpl = nextD()
                for j in range(8):
                    pg.op("pe", lambda: T.matmul(pl[:, 0:nt], onesF[:, :], yT[:, j, :nt], start=(j == 0), stop=(j == 7)), reads=[ryT, rC], writes=[rpl], inc=(j == 7))
                for j in range(8):
                    pg.op("pe", lambda: T.matmul(pl[:, 128:128 + nt], onesF[:, :], ysq[:, j, :nt], start=(j == 0), stop=(j == 7)), reads=[rysq, rC], writes=[rpl], inc=(j == 7))
                pg.op("dve", lambda: V.tensor_scalar(lnst[:, 0, :nt], pl[:, 0:nt], 1.0 / CCH, None, op0=ALU.mult), reads=[rpl], writes=[rln])
                pg.op("dve", lambda: V.tensor_scalar(lnst[:, 1, :nt], pl[:, 128:128 + nt], 1.0 / CCH, None, op0=ALU.mult), reads=[rpl], writes=[rln])
                pg.op("dve", lambda: V.tensor_tensor(lnst[:, 2, :nt], lnst[:, 0, :nt], lnst[:, 0, :nt], op=ALU.mult), reads=[rln], writes=[rln])
                pg.op("dve", lambda: V.tensor_tensor(lnst[:, 1, :nt], lnst[:, 1, :nt], lnst[:, 2, :nt], op=ALU.subtract), reads=[rln], writes=[rln])
                pg.op("act", lambda: A.activation(lnst[:, 2, :nt], lnst[:, 1, :nt], AF.Sqrt, bias=EPS, scale=1.0), reads=[rln], writes=[rln])
                pg.op("dve", lambda: V.reciprocal(lnst[:, 3, :nt], lnst[:, 2, :nt]), reads=[rln], writes=[rln])
                mb = lnst[:, 0:1, :nt].to_broadcast([128, 8, nt]); rb_ = lnst[:, 3:4, :nt].to_broadcast([128, 8, nt])
                pg.op("dve", lambda: V.tensor_tensor(ysq[:, :, :nt], yT[:, :, :nt], mb, op=ALU.subtract), reads=[ryT, rln], writes=[rysq])
                pg.op("dve", lambda: V.tensor_tensor(ysq[:, :, :nt], ysq[:, :, :nt], rb_, op=ALU.mult), reads=[rysq, rln], writes=[rysq])
                for j in range(8):
                    pg.op("act", lambda: A.activation(ao[:, 8 + j, :nt], ysq[:, j, :nt], AF.Silu, bias=cpar[:, 2, j:j + 1], scale=cpar[:, 1, j:j + 1]), reads=[rysq, rC], writes=[rao])
                pg.dma("sp", "acs%d" % (t % 2), ACs[t, :, :, :nt], ao[:, :, :nt], reads=[rao])
                if t < NTP - 1:
                    pg.op("pool", lambda: G.tensor_copy(uh[:, :, 0:30], uh[:, :, 128:158]), reads=[ruh], writes=[ruh])
        pg.barrier()
        if nphase < 2:
            return nc, dbgo

        with ExitStack() as s2:
            Wout = SB(s2, "Wout", [128, DC, D], BF16); rWout = Res("Wout")
            wov = w_out.rearrange("(kc p) n -> p kc n", p=128)
            for c in range(4):
                pg.dma("pool", "wout", Wout[:, :, c * 512:(c + 1) * 512], wov[:, :, c * 512:(c + 1) * 512], writes=[rWout])
            wr = SB(s2, "wr", [128, DC, NE]); brs = SB(s2, "brs", [1, NE]); tri = SB(s2, "tri", [128, 128])
            iotaE = SB(s2, "iotaE", [128, NE]); iotaC = SB(s2, "iotaC", [128, NE])
            for dst, src in ((wr, wrT), (brs, brow), (tri, triF_d), (iotaE, iotaE_d), (iotaC, iotaC_d)):
                pg.dma("sp", "c2b", dst[:], src, writes=[rC])
            g1bc = SB(s2, "g1bc", [128, D]); G2bc = SB(s2, "G2bc", [128, D]); S2bc = SB(s2, "S2bc", [128, D]); rbc = Res("bc")
            def load_bc(s):
                for dst, i in ((g1bc, 2), (G2bc, 4), (S2bc, 3)):
                    pg.dma("sp", "bc", dst[:], mrow(s, i).to_broadcast([128, D]), reads=[rMODS], writes=[rbc])
            load_bc(0)
            acb = [SB(s2, "acb%d" % i, [128, 16, 128], BF16) for i in range(2)]; racb = [Res("acb%d" % i) for i in range(2)]
            xb = [SB(s2, "xb2%d" % i, [128, D]) for i in range(2)]; rxb = [Res("xb2%d" % i) for i in range(2)]
            tmp = SB(s2, "tmp", [128, D]); rtmp = Res("tmp")
            x1b = [SB(s2, "x1b%d" % i, [128, D]) for i in range(2)]; rx1 = [Res("x1b%d" % i) for i in range(2)]
            h2a = SB(s2, "h2a", [128, D]); rh2a = Res("h2a")
            h2 = SB(s2, "h2", [128, D]); rh2 = Res("h2")
            h2b = [SB(s2, "h2b%d" % i, [128, D], BF16) for i in range(2)]; rh2b = [Res("h2b%d" % i) for i in range(2)]
            h2T = SB(s2, "h2T", [128, DC, 128]); rh2T = Res("h2T")
            st2 = SB(s2, "st2", [128, 16]); rst2 = Res("st2")
            lg = SB(s2, "lg", [128, NE]); top8 = SB(s2, "top8", [128, 8]); idx8 = SB(s2, "idx8", [128, 8], U32); idxf = SB(s2, "idxf", [128, 8])
            Mk = SB(s2, "Mk", [128, NE]); pos = SB(s2, "pos", [128, NE]); val = SB(s2, "val", [128, NE]); oh = SB(s2, "oh", [128, NE]); oh2 = SB(s2, "oh2", [128, NE])
            cntbc = SB(s2, "cntbc", [128, NE]); dstf = SB(s2, "dstf", [128, 4]); rr = Res("router")
            pg.op("dve", lambda: V.memset(cntbc[:], 0.0), writes=[rr])
            pg.op("dve", lambda: V.memset(DEST[:, NTP, :], 1 << 30), writes=[rDEST])
            def load_t(t):
                src, nt, s = xrows(t)
                pg.dma("sp", "acb%d" % (t % 2), acb[t % 2][:, :, :nt], ACs[t, :, :, :nt], writes=[racb[t % 2]])
                pg.dma("sp", "xb2%d" % (t % 2), xb[t % 2][:nt, :], src, writes=[rxb[t % 2]])
            load_t(0)
            for t in range(min(NTILE, tlim)):
                src, nt, s = xrows(t)
                if t == NTP:
                    load_bc(1)
                if t + 1 < NTILE:
                    load_t(t + 1)
                ab = acb[t % 2]; rab = racb[t % 2]; x = xb[t % 2]; rx = rxb[t % 2]; x1 = x1b[t % 2]; rx1_ = rx1[t % 2]
                for ng in range(4):
                    if ng % 2 == 0:
                        pw, rpw = nextD()
                    for c in range(DC):
                        pg.op("pe", lambda: T.matmul(pw[:nt, (ng % 2) * 512:(ng % 2 + 1) * 512], ab[:, c, :nt], Wout[:, c, ng * 512:(ng + 1) * 512], start=(c == 0), stop=(c == DC - 1)),
                              reads=[rab, rWout], writes=[rpw], inc=(c == DC - 1))
                    pg.op("dve", lambda: V.tensor_tensor(tmp[:nt, ng * 512:(ng + 1) * 512], pw[:nt, (ng % 2) * 512:(ng % 2 + 1) * 512], g1bc[:nt, ng * 512:(ng + 1) * 512], op=ALU.mult),
                          reads=[rpw, rbc], writes=[rtmp])
                pg.op("pool", lambda: G.tensor_tensor(x1[:nt, :], tmp[:nt, :], x[:nt, :], op=ALU.add), reads=[rtmp, rx], writes=[rx1_])
                xr0 = t * 128
                pg.dma("sp", "x1st%d" % (t % 2), X1[xr0:xr0 + nt, :], x1[:nt, :], reads=[rx1_])
                pg.op("dve", lambda: V.memset(st2[:nt, 0:1], 0.0), writes=[rst2])
                pg.op("act", lambda: A.activation(h2a[:nt, :], x1[:nt, :], AF.Square, accum_out=st2[:nt, 0:1]), reads=[rx1_], writes=[rh2a, rst2])
                pg.op("act", lambda: A.activation(st2[:nt, 1:2], st2[:nt, 0:1], AF.Sqrt, bias=EPS, scale=1.0 / D), reads=[rst2], writes=[rst2])
                pg.op("dve", lambda: V.reciprocal(st2[:nt, 2:3], st2[:nt, 1:2]), reads=[rst2], writes=[rst2])
                pg.op("dve", lambda: V.scalar_tensor_tensor(h2a[:nt, :], x1[:nt, :], st2[:nt, 2:3], G2bc[:nt, :], op0=ALU.mult, op1=ALU.mult), reads=[rx1_, rst2, rbc], writes=[rh2a])
                pg.op("pool", lambda: G.tensor_tensor(h2[:nt, :], h2a[:nt, :], S2bc[:nt, :], op=ALU.add), reads=[rh2a, rbc], writes=[rh2])
                hb_ = h2b[t % 2]; rhb_ = rh2b[t % 2]
                pg.op("act", lambda: A.copy(hb_[:nt, :], h2[:nt, :]), reads=[rh2], writes=[rhb_])
                for half in range(2):
                    pt, rpt = nextD()
                    for b8 in range(8):
                        dc = half * 8 + b8
                        pg.op("pe", lambda: T.transpose(pt[:, b8 * 128:b8 * 128 + nt], h2[:nt, dc * 128:(dc + 1) * 128], identF[:nt, :nt]), reads=[rh2, rC], writes=[rpt], inc=(b8 == 7))
                    pg.op("act", lambda: A.copy(h2T[:, half * 8:(half + 1) * 8, :nt], pt[:, :].rearrange("p (b k) -> p b k", k=128)[:, :, :nt]), reads=[rpt], writes=[rh2T])
                pl, rpl = nextD()
                for dc in range(DC):
                    pg.op("pe", lambda: T.matmul(pl[:nt, 0:NE], h2T[:, dc, :nt], wr[:, dc, :], start=(dc == 0), stop=False), reads=[rh2T, rC], writes=[rpl], inc=False)
                pg.op("pe", lambda: T.matmul(pl[:nt, 0:NE], onesF[0:1, :nt], brs[0:1, :], start=False, stop=True), reads=[rC], writes=[rpl])
                pg.op("dve", lambda: V.tensor_copy(lg[:nt, :], pl[:nt, 0:NE]), reads=[rpl], writes=[rr])
                pg.op("dve", lambda: V.max(out=top8[:nt, :], in_=lg[:nt, :]), reads=[rr], writes=[rr])
                pg.op("dve", lambda: V.max_index(out=idx8[:nt, :], in_max=top8[:nt, :], in_values=lg[:nt, :]), reads=[rr], writes=[rr])
                pg.op("dve", lambda: V.tensor_scalar(st2[:nt, 4:5], top8[:nt, 0:1], -1.0, None, op0=ALU.mult), reads=[rr], writes=[rst2])
                pg.op("dve", lambda: V.memset(st2[:nt, 5:6], 0.0), writes=[rst2])
                pg.op("act", lambda: A.activation(st2[:nt, 8:12], top8[:nt, 0:4], AF.Exp, bias=st2[:nt, 4:5], scale=1.0, accum_out=st2[:nt, 5:6]), reads=[rr, rst2], writes=[rst2])
                pg.op("dve", lambda: V.reciprocal(st2[:nt, 6:7], st2[:nt, 5:6]), reads=[rst2], writes=[rst2])
                pg.op("dve", lambda: V.tensor_scalar(GATES[:nt, t, :], st2[:nt, 8:12], st2[:nt, 6:7], None, op0=ALU.mult), reads=[rst2], writes=[rGATES])
                pg.op("dve", lambda: V.tensor_scalar(Mk[:nt, :], lg[:nt, :], top8[:nt, 3:4], None, op0=ALU.is_ge), reads=[rr], writes=[rr])
                pc, rpc = nextD()
                pg.op("pe", lambda: T.matmul(pc[:nt, 0:NE], tri[:nt, :nt], Mk[:nt, :], start=True, stop=True), reads=[rr, rC], writes=[rpc])
                pg.op("pe", lambda: T.matmul(pc[:, 512:512 + NE], onesF[:nt, :], Mk[:nt, :], start=True, stop=True), reads=[rr, rC], writes=[rpc])
                pg.op("dve", lambda: V.tensor_tensor(pos[:nt, :], pc[:nt, 0:NE], cntbc[:nt, :], op=ALU.add), reads=[rpc, rr], writes=[rr])
                pg.op("dve", lambda: V.tensor_tensor(cntbc[:, :], pc[:, 512:512 + NE], cntbc[:, :], op=ALU.add), reads=[rpc, rr], writes=[rr])
                pg.op("dve", lambda: V.scalar_tensor_tensor(val[:nt, :], pos[:nt, :], float(cap - 1), iotaC[:nt, :], op0=ALU.min, op1=ALU.add), reads=[rr, rC], writes=[rr])
                pg.op("dve", lambda: V.tensor_copy(idxf[:nt, 0:4], idx8[:nt, 0:4]), reads=[rr], writes=[rr])
                for k in range(TOPK):
                    pg.op("dve", lambda: V.tensor_scalar(oh[:nt, :], iotaE[:nt, :], idxf[:nt, k:k + 1], None, op0=ALU.is_equal), reads=[rr, rC], writes=[rr])
                    pg.op("dve", lambda: V.tensor_tensor(oh2[:nt, :], oh[:nt, :], val[:nt, :], op=ALU.mult), reads=[rr], writes=[rr])
                    pg.op("dve", lambda: V.tensor_reduce(dstf[:nt, k:k + 1], oh2[:nt, :], axis=AX.X, op=ALU.add), reads=[rr], writes=[rr])
                pg.op("dve", lambda: V.tensor_copy(DEST[:nt, t, :], dstf[:nt, 0:4]), reads=[rr], writes=[rDEST])
                for k in range(TOPK):
                    pg.idma("xs", XS, bass.IndirectOffsetOnAxis(ap=DEST[:, t, k:k + 1], axis=0), hb_[:, :], None,
                            reads=[rhb_, rDEST], bounds_check=BCV, oob_is_err=False)
            if dbg:
                pg.dma("sp", "dbg", dbg_dest, DEST[:].rearrange("p t k -> p (t k)"), reads=[rDEST])
                pg.dma("sp", "dbg", dbg_gate, GATES[:].rearrange("p t k -> p (t k)"), reads=[rGATES])
        pg.barrier()
        if nphase < 3:
            return nc, dbgo

        NG = cap // 512; NSL = cap // 128
        with ExitStack() as s3:
            XT = SB(s3, "XT", [128, DC, cap], BF16); rXT = Res("XT")
            AT = SB(s3, "AT", [128, DC, cap], BF16); rAT = Res("AT")
            NW = 4
            wsl = [SB(s3, "wch%d" % i, [128, DC, 512], BF16) for i in range(NW)]; rws = [Res("wch%d" % i) for i in range(NW)]
            bg = SB(s3, "bg", [128, NE, 32]); pg.dma("sp", "c3", bg[:], bguT, writes=[rC])
            bdb = [SB(s3, "bdb%d" % i, [128, D]) for i in range(2)]; rbdb = [Res("bdb%d" % i) for i in range(2)]
            gcs = [SB(s3, "gc%d" % i, [128, 512]) for i in range(2)]; sgs = [SB(s3, "sgm%d" % i, [128, 512]) for i in range(2)]
            ucs = [SB(s3, "uc%d" % i, [128, 512]) for i in range(2)]; rsw = [Res("sw%d" % i) for i in range(2)]
            yst = [SB(s3, "yst%d" % i, [128, 512]) for i in range(4)]; ryst = [Res("yst%d" % i) for i in range(4)]
            chunks = []
            for e in range(NE):
                for i in range(4):
                    chunks.append((e, "g", i)); chunks.append((e, "u", i))
                for dn in range(4):
                    chunks.append((e, "d", dn))
            issued = [0]
            def issue_upto(ci):
                while issued[0] <= min(ci, len(chunks) - 1):
                    c = issued[0]; e, kind, i = chunks[c]; sl = c % NW
                    if kind == "d":
                        srcv = w_down[e].rearrange("(kc p) n -> p kc n", p=128)[:, :, i * 512:(i + 1) * 512]
                    else:
                        off = 0 if kind == "g" else DFF
                        srcv = w_gu[e].rearrange("(kc p) n -> p kc n", p=128)[:, :, off + i * 512: off + (i + 1) * 512]
                    pg.dma("pool", "wch%d" % sl, wsl[sl][:], srcv, writes=[rws[sl]])
                    issued[0] += 1
            ci = 0; sw_i = 0; y_i = 0
            for e in range(NE):
                for dc in range(DC):
                    pg.dma("sp", "xt", XT[:, dc, :], XS[e * cap:(e + 1) * cap, dc * 128:(dc + 1) * 128], writes=[rXT], transpose=True)
                pg.dma("sp", "bdb%d" % (e % 2), bdb[e % 2][:], b_down[e:e + 1, :].to_broadcast([128, D]), writes=[rbdb[e % 2]])
                for i in range(4):
                    issue_upto(ci + 3)
                    wg = wsl[ci % NW]; rwg = rws[ci % NW]; wu = wsl[(ci + 1) % NW]; rwu = rws[(ci + 1) % NW]
                    for fl in range(4):
                        fc = 4 * i + fl
                        for sgi in range(NG):
                            pgu, rpgu = nextD()
                            for which, (wt_, rwt_) in enumerate(((wg, rwg), (wu, rwu))):
                                for dc in range(DC):
                                    pg.op("pe", lambda: T.matmul(pgu[:, which * 512:(which + 1) * 512], wt_[:, dc, fl * 128:(fl + 1) * 128], XT[:, dc, sgi * 512:(sgi + 1) * 512], start=(dc == 0), stop=(dc == DC - 1)),
                                          reads=[rwt_, rXT], writes=[rpgu], inc=(dc == DC - 1))
                            k2 = sw_i % 2; sw_i += 1
                            gc = gcs[k2]; sgm = sgs[k2]; uc = ucs[k2]; rs_ = rsw[k2]
                            pg.op("dve", lambda: V.tensor_scalar(gc[:], pgu[:, 0:512], bg[:, e, fc:fc + 1], 7.0, op0=ALU.add, op1=ALU.min), reads=[rpgu, rC], writes=[rs_])
                            pg.op("act", lambda: A.activation(sgm[:], gc[:], AF.Sigmoid, scale=1.702), reads=[rs_], writes=[rs_])
                            pg.op("dve", lambda: V.tensor_scalar(uc[:], pgu[:, 512:1024], bg[:, e, 16 + fc:17 + fc], 7.0, op0=ALU.add, op1=ALU.min), reads=[rpgu, rC], writes=[rs_])
                            pg.op("dve", lambda: V.tensor_scalar(uc[:], uc[:], -7.0, 1.0, op0=ALU.max, op1=ALU.add), reads=[rs_], writes=[rs_])
                            pg.op("dve", lambda: V.tensor_tensor(gc[:], gc[:], sgm[:], op=ALU.mult), reads=[rs_], writes=[rs_])
                            pg.op("dve", lambda: V.tensor_tensor(AT[:, fc, sgi * 512:(sgi + 1) * 512], gc[:], uc[:], op=ALU.mult), reads=[rs_], writes=[rAT])
                    ci += 2
                for dn in range(4):
                    issue_upto(ci + 3)
                    wd = wsl[ci % NW]; rwd = rws[ci % NW]
                    for stl in range(NSL):
                        if stl % 2 == 0:
                            pdn, rpdn = nextD()
                        po_ = pdn[:, (stl % 2) * 512:(stl % 2 + 1) * 512]
                        for fc in range(DC):
                            pg.op("pe", lambda: T.matmul(po_, AT[:, fc, stl * 128:(stl + 1) * 128], wd[:, fc, :], start=(fc == 0), stop=(fc == DC - 1)),
                                  reads=[rAT, rwd], writes=[rpdn], inc=(fc == DC - 1))
                        ys = yst[y_i % 4]; rys = ryst[y_i % 4]; y_i += 1
                        pg.op("dve", lambda: V.tensor_tensor(ys[:], po_, bdb[e % 2][:, dn * 512:(dn + 1) * 512], op=ALU.add), reads=[rpdn, rbdb[e % 2]], writes=[rys])
                        r0 = e * cap + stl * 128
                        pg.dma("sp", "yst%d" % ((y_i - 1) % 4), YSh[dn // 2][r0:r0 + 128, (dn % 2) * 512:(dn % 2 + 1) * 512], ys[:], reads=[rys])
                    ci += 1
        pg.barrier()
        if nphase < 4:
            return nc, dbgo

        with ExitStack() as s4:
            g2bc = SB(s4, "g2bc", [128, D]); gfbc = SB(s4, "gfbc", [128, D]); rbc3 = Res("bc3")
            pg.dma("sp", "bc3", g2bc[:], mrow(0, 5).to_broadcast([128, D]), reads=[rMODS], writes=[rbc3])
            pg.dma("sp", "bc3", gfbc[:], gfin.to_broadcast([128, D]), writes=[rbc3])
            Gk = [[[SB(s4, "G%d_%d_%d" % (st_, k, hf), [128, D // 2]) for hf in range(2)] for k in range(TOPK)] for st_ in range(2)]
            rGk = [[Res("G%d_%d" % (st_, k)) for k in range(TOPK)] for st_ in range(2)]
            x1t = [SB(s4, "x1t%d" % i, [128, D]) for i in range(2)]; rx1t = [Res("x1t%d" % i) for i in range(2)]
            acc = SB(s4, "acc", [128, D]); racc = Res("acc")
            yb = [SB(s4, "yb%d" % i, [128, D]) for i in range(2)]; ryb = [Res("yb%d" % i) for i in range(2)]
            st3 = SB(s4, "st3", [128, 8]); rst3 = Res("st3")
            def load3(t):
                src, nt, s = xrows(t)
                for k in range(TOPK):
                    for hf in range(2):
                        pg.idma("g%d_%d_%d" % (t % 2, k, hf), Gk[t % 2][k][hf][:, :], None, YSh[hf], bass.IndirectOffsetOnAxis(ap=DEST[:, t, k:k + 1], axis=0),
                                reads=[rDEST], writes=[rGk[t % 2][k]], bounds_check=BCV, oob_is_err=False)
                pg.dma("sp", "x1t%d" % (t % 2), x1t[t % 2][:nt, :], X1[t * 128:t * 128 + nt, :], writes=[rx1t[t % 2]])
            load3(0)
            for t in range(min(NTILE, tlim)):
                src, nt, s = xrows(t)
                if t == NTP:
                    pg.dma("sp", "bc3", g2bc[:], mrow(1, 5).to_broadcast([128, D]), reads=[rMODS], writes=[rbc3])
                if t + 1 < NTILE:
                    load3(t + 1)
                Gs = Gk[t % 2]; rGs = rGk[t % 2]; x1 = x1t[t % 2]; rx1_ = rx1t[t % 2]; y = yb[t % 2]; ry = ryb[t % 2]
                for hf in range(2):
                    cs_ = slice(hf * 1024, (hf + 1) * 1024)
                    pg.op("dve", lambda: V.tensor_scalar(acc[:nt, cs_], Gs[0][hf][:nt, :], GATES[:nt, t, 0:1], None, op0=ALU.mult), reads=[rGs[0], rGATES], writes=[racc])
                    for k in range(1, TOPK):
                        pg.op("dve", lambda: V.scalar_tensor_tensor(acc[:nt, cs_], Gs[k][hf][:nt, :], GATES[:nt, t, k:k + 1], acc[:nt, cs_], op0=ALU.mult, op1=ALU.add), reads=[rGs[k], rGATES, racc], writes=[racc])
                pg.op("dve", lambda: V.tensor_tensor(acc[:nt, :], acc[:nt, :], g2bc[:nt, :], op=ALU.mult), reads=[racc, rbc3], writes=[racc])
                pg.op("pool", lambda: G.tensor_tensor(y[:nt, :], acc[:nt, :], x1[:nt, :], op=ALU.add), reads=[racc, rx1_], writes=[ry])
                pg.op("dve", lambda: V.memset(st3[:nt, 0:1], 0.0), writes=[rst3])
                pg.op("act", lambda: A.activation(acc[:nt, :], y[:nt, :], AF.Square, accum_out=st3[:nt, 0:1]), reads=[ry], writes=[racc, rst3])
                pg.op("act", lambda: A.activation(st3[:nt, 1:2], st3[:nt, 0:1], AF.Sqrt, bias=EPS, scale=1.0 / D), reads=[rst3], writes=[rst3])
                pg.op("dve", lambda: V.reciprocal(st3[:nt, 2:3], st3[:nt, 1:2]), reads=[rst3], writes=[rst3])
                pg.op("dve", lambda: V.scalar_tensor_tensor(y[:nt, :], y[:nt, :], st3[:nt, 2:3], gfbc[:nt, :], op0=ALU.mult, op1=ALU.mult), reads=[ry, rst3, rbc3], writes=[ry])
                dsto = y_p[t * 128:(t + 1) * 128, :] if t < NTP else y_s[:, :]
                pg.dma("sp", "yo%d" % (t % 2), dsto, y[:nt, :], reads=[ry])
        pg.barrier()
    return nc, dbgo


def _consts():
    identF = np.eye(128, dtype=np.float32)
    tri = (np.arange(128)[:, None] < np.arange(128)[None, :]).astype(np.float32)
    iotaE = np.broadcast_to(np.arange(NE, dtype=np.float32)[None, :], (128, NE)).copy()
    m = np.zeros((128, 4, 256), np.float32)
    q = np.arange(128)[:, None]; k = np.arange(128)[None, :]
    cur = np.where((q < 64) & (k >= 64), NEGM, 0.0)
    prev = np.where((q >= 64) & (k < 64), NEGM, 0.0)
    m[:, 0, 0:128] = cur; m[:, 0, 128:256] = prev
    m[:, 1, 128:256] = cur; m[:, 1, 0:128] = prev
    m[:, 2, 0:128] = cur; m[:, 2, 128:256] = NEGM
    m[:, 3, 0:128] = np.where(k >= NS, NEGM, 0.0); m[:, 3, 128:256] = 0.0
    inv_freq = (np.float32(500000.0) ** (-np.arange(0, 16, 2, dtype=np.float32) / np.float32(16))).astype(np.float32)
    pos = np.zeros((NTILE, 128), np.float32)
    pos[:NTP] = np.arange(SEQ, dtype=np.float32).reshape(NTP, 128)
    pos[NTP, :NS] = 2048 + np.arange(NS, dtype=np.float32)
    ang = (pos[:, :, None] * inv_freq[None, None, :]).astype(np.float32)
    rope = np.concatenate([np.cos(ang), np.sin(ang)], axis=-1).astype(np.float32)
    return dict(identF=identF, identB=identF.astype(ml_dtypes.bfloat16), onesF=np.ones((128, 128), np.float32), triF=tri,
                iotaE=iotaE, masks=m.astype(ml_dtypes.bfloat16), ropeT=np.ascontiguousarray(rope.transpose(1, 0, 2)))


def make_in_maps(inputs, cap=CAP, cores=range(8), small_w=False):
    f = lambda a: np.ascontiguousarray(np.asarray(a, dtype=np.float32))
    cst = _consts()
    cst["iotaC"] = cst["iotaE"] * np.float32(cap)
    shared = dict(
        w_ada=f(inputs["w_ada"][0]), bada2=f(np.broadcast_to(inputs["b_ada"][0][None, :], (2, 6 * D))),
        gmix2=f(np.broadcast_to(inputs["g_mix"][0][None, :], (2, D))), gffn2=f(np.broadcast_to(inputs["g_ffn"][0][None, :], (2, D))),
        gfin=f(inputs["g_final"][None, :]), w_in=f(inputs["w_in"][0]), w_out=f(inputs["w_out"][0]),
        w_gu=(np.zeros((1, 8, 8), np.float32) if small_w else f(inputs["w_gu"][0])), w_down=(np.zeros((1, 8, 8), np.float32) if small_w else f(inputs["w_down"][0])),
        bguT=f(np.asarray(inputs["b_gu"][0]).reshape(NE, 32, 128).transpose(2, 0, 1)), b_down=f(inputs["b_down"][0]),
        wrT=f(np.asarray(inputs["w_router"][0]).reshape(DC, 128, NE).transpose(1, 0, 2)), brow=f(inputs["b_router"][0][None, :]),
        sinkbc=f(np.broadcast_to(inputs["attn_sink"][0][None, :], (128, 16))),
        cwT=f(np.asarray(inputs["conv_w"][0])[:, 0, :].reshape(31, 8, 128).transpose(2, 1, 0)),
        convp=f(np.stack([np.asarray(inputs[k][0]).reshape(8, 128).T for k in ("conv_b", "conv_ln_g", "conv_ln_b")], axis=1)),
        **cst)
    maps = []
    for b in cores:
        c2 = np.stack([np.asarray(inputs["c_prompt"][b]), np.asarray(inputs["c_sample"][b])], 0)
        m = dict(shared)
        m.update(xp=f(inputs["x_prompt"][b]), xsm=f(inputs["x_sample"][b]),
                 c2T=f(c2.reshape(2, DC, 128).transpose(2, 1, 0)),
                 ck=f(np.asarray(inputs["cache_k"][0, b]).reshape(128, 256)), cvv=f(np.asarray(inputs["cache_v"][0, b]).reshape(128, 256)),
                 sconvT=f(np.asarray(inputs["state_conv"][0, b]).T.reshape(8, 128, 30).transpose(1, 0, 2)))
        maps.append(m)
    return maps


_NC = {}
def kernel(**inputs):
    if "nc" not in _NC:
        _NC["nc"] = build()[0]
    nc = _NC["nc"]
    maps = make_in_maps(inputs)
    res = run_bass_kernel_spmd(nc, maps, core_ids=list(range(8)))
    R = [{k: np.asarray(v) for k, v in r.items()} for r in res.results]
    st = lambda k: np.stack([r[k] for r in R], 0)
    y_prompt = st("y_p").astype(np.float32); y_sample = st("y_s").astype(np.float32)
    nkp = st("nkp").reshape(1, 8, 128, 4, 64); nvp = st("nvp").reshape(1, 8, 128, 4, 64)
    ncp = st("ncp").reshape(1, 8, 30, CCH)
    nks = st("nks").reshape(1, 8, NS, 4, 64); nvs = st("nvs").reshape(1, 8, NS, 4, 64)
    ncs = st("ncs").reshape(1, 8, 30, CCH)
    return (y_prompt, y_sample, nkp.astype(np.float32), nvp.astype(np.float32), ncp.astype(np.float32),
            nks.astype(np.float32), nvs.astype(np.float32), ncs.astype(np.float32))
```
